# Optimizing a Trainium2 kernel written in Bass

```python
import math
import jax, jax.numpy as jnp
from jax import lax
import numpy as np

D_MODEL = 1024
BATCH = 32
SEQ = 2048
DEPTH = 4

N_MIXERS = 2
N_ATTN_LAYERS = (DEPTH + N_MIXERS - 1) // N_MIXERS
N_REC_LAYERS = DEPTH // N_MIXERS
HEAD_DIM = 64
ATTN_HEADS = D_MODEL // (2 * HEAD_DIM)
ATTN_WIDTH = ATTN_HEADS * 2 * HEAD_DIM
Q_BLOCK = 128
SUBLN_EPS = 1e-5
D_RNN = D_MODEL
RNN_HEADS = 4
RNN_BLOCK = D_RNN // RNN_HEADS
CONV_WIDTH = 4
RG_LRU_C = 8.0
MIN_RAD = 0.9
MAX_RAD = 0.999
N_GROUPS = 4
EXPERTS_PER_GROUP = 4
N_EXPERTS = N_GROUPS * EXPERTS_PER_GROUP
TOP_K = 2
D_EXPERT = 512
NORM_EPS = 1e-6

kernel_name = "hybrid_diffattn_rglru_hmoe_adaln"


def rms_norm(x, g, eps=NORM_EPS):
    xf = x.astype(jnp.float32)
    y = xf * lax.rsqrt(jnp.mean(xf * xf, axis=-1, keepdims=True) + eps)
    return (y * g.astype(jnp.float32)).astype(x.dtype)


def modulate(h, shift, scale):
    return h * (1 + scale[:, None, :]) + shift[:, None, :]


def lambda_init_fn(layer):
    return 0.8 - 0.6 * math.exp(-0.3 * layer)


def diff_attention(h, w_qkv, lam_vec, subln_g, w_o, lambda_init):
    B, S, _ = h.shape
    qkv = h @ w_qkv
    q, k, v = jnp.split(qkv, 3, axis=-1)
    q = q.reshape(B, S, ATTN_HEADS, 2, HEAD_DIM)
    k = k.reshape(B, S, ATTN_HEADS, 2, HEAD_DIM)
    v = v.reshape(B, S, ATTN_HEADS, 2 * HEAD_DIM)
    lv = lam_vec.astype(jnp.float32)
    lam = jnp.exp(jnp.sum(lv[0] * lv[1])) - jnp.exp(jnp.sum(lv[2] * lv[3])) + lambda_init
    scale = HEAD_DIM ** -0.5
    outs = []
    for blk in range(S // Q_BLOCK):
        q0 = blk * Q_BLOCK
        kv_len = q0 + Q_BLOCK
        s = jnp.einsum('bqhnd,bkhnd->bhnqk', q[:, q0:kv_len], k[:, :kv_len]).astype(jnp.float32) * scale
        mask = (q0 + jnp.arange(Q_BLOCK))[:, None] >= jnp.arange(kv_len)[None, :]
        p = jax.nn.softmax(jnp.where(mask, s, -jnp.inf), axis=-1)
        p = p[:, :, 0] - lam * p[:, :, 1]
        outs.append(jnp.einsum('bhqk,bkhe->bqhe', p.astype(v.dtype), v[:, :kv_len]))
    o = jnp.concatenate(outs, axis=1)
    o = rms_norm(o, subln_g, SUBLN_EPS) * (1 - lambda_init)
    return o.reshape(B, S, ATTN_WIDTH) @ w_o


def causal_depthwise_conv(x, w, b):
    y = lax.conv_general_dilated(x, w[:, None, :], window_strides=(1,), padding=[(CONV_WIDTH - 1, 0)],
                                 dimension_numbers=('NWC', 'WIO', 'NWC'), feature_group_count=x.shape[-1])
    return y + b


def _linear_combine(left, right):
    a1, b1 = left
    a2, b2 = right
    return a1 * a2, a2 * b1 + b2


def recurrent_block(h, w_in, conv_w, conv_b, gate_w, gate_b, a_param, w_out):
    B, S, _ = h.shape
    y, xr = jnp.split(h @ w_in, 2, axis=-1)
    xr = causal_depthwise_conv(xr, conv_w, conv_b)
    gates = jnp.einsum('bshi,hij->bshj', xr.reshape(B, S, RNN_HEADS, RNN_BLOCK), gate_w) + gate_b
    gates = jax.nn.sigmoid(gates.astype(jnp.float32))
    r = gates[..., :RNN_BLOCK].reshape(B, S, D_RNN)
    i = gates[..., RNN_BLOCK:].reshape(B, S, D_RNN)
    log_a = -RG_LRU_C * r * jax.nn.softplus(-a_param.astype(jnp.float32))
    a = jnp.exp(log_a)
    b = jnp.sqrt(-jnp.expm1(2 * log_a)) * (i * xr.astype(jnp.float32))
    _, hs = lax.associative_scan(_linear_combine, (a, b), axis=1)
    return (jax.nn.gelu(y, approximate=True) * hs.astype(y.dtype)) @ w_out


def hier_moe(h, w_group, b_group, w_expert, b_expert, w13, w2):
    B, S, D = h.shape
    t = h.reshape(-1, D)
    g_logits = (t @ w_group).astype(jnp.float32) + b_group
    g_idx = jnp.argmax(g_logits, axis=-1)
    g_w = jnp.take_along_axis(jax.nn.softmax(g_logits, axis=-1), g_idx[:, None], axis=-1)[:, 0]
    e_logits = ((t @ w_expert).astype(jnp.float32) + b_expert).reshape(-1, N_GROUPS, EXPERTS_PER_GROUP)
    e_in = jnp.take_along_axis(e_logits, g_idx[:, None, None], axis=1)[:, 0]
    top_v, top_i = lax.top_k(e_in, TOP_K)
    top_w = jax.nn.softmax(top_v, axis=-1) * g_w[:, None]
    expert_id = g_idx[:, None] * EXPERTS_PER_GROUP + top_i
    combine = jnp.sum(jax.nn.one_hot(expert_id, N_EXPERTS, dtype=jnp.float32) * top_w[..., None], axis=1)
    combine = combine.astype(t.dtype)
    out = jnp.zeros_like(t)
    for e in range(N_EXPERTS):
        u = t @ w13[e]
        act = jax.nn.silu(u[:, :D_EXPERT]) * u[:, D_EXPERT:]
        out = out + combine[:, e:e + 1] * (act @ w2[e])
    return out.reshape(B, S, D)


def setup_inputs(seed: int = 0) -> dict:
    key = jax.random.key(seed)
    ks = jax.random.split(key, 24)
    f32 = jnp.float32

    def nrm(k, shape, scale):
        return jax.random.normal(k, shape, f32) * scale

    u = jax.random.uniform(ks[16], (N_REC_LAYERS, D_RNN), f32, MIN_RAD ** 2, MAX_RAD ** 2)
    return {
        "x": nrm(ks[0], (BATCH, SEQ, D_MODEL), 1.0),
        "c": nrm(ks[1], (BATCH, D_MODEL), 1.0),
        "norm_mix": 1.0 + nrm(ks[2], (DEPTH, D_MODEL), 0.02),
        "norm_ffn": 1.0 + nrm(ks[3], (DEPTH, D_MODEL), 0.02),
        "final_norm": 1.0 + nrm(ks[4], (D_MODEL,), 0.02),
        "ada_w": nrm(ks[5], (DEPTH, D_MODEL, 6 * D_MODEL), 0.5 * D_MODEL ** -0.5),
        "ada_b": nrm(ks[6], (DEPTH, 6 * D_MODEL), 0.02),
        "attn_w_qkv": nrm(ks[7], (N_ATTN_LAYERS, D_MODEL, 3 * ATTN_WIDTH), D_MODEL ** -0.5),
        "attn_lambda": nrm(ks[8], (N_ATTN_LAYERS, 4, HEAD_DIM), 0.1),
        "attn_subln": 1.0 + nrm(ks[9], (N_ATTN_LAYERS, 2 * HEAD_DIM), 0.02),
        "attn_w_o": nrm(ks[10], (N_ATTN_LAYERS, ATTN_WIDTH, D_MODEL), ATTN_WIDTH ** -0.5),
        "rec_w_in": nrm(ks[11], (N_REC_LAYERS, D_MODEL, 2 * D_RNN), D_MODEL ** -0.5),
        "rec_conv_w": nrm(ks[12], (N_REC_LAYERS, CONV_WIDTH, D_RNN), CONV_WIDTH ** -0.5),
        "rec_conv_b": nrm(ks[13], (N_REC_LAYERS, D_RNN), 0.01),
        "rec_gate_w": nrm(ks[14], (N_REC_LAYERS, RNN_HEADS, RNN_BLOCK, 2 * RNN_BLOCK), RNN_BLOCK ** -0.5),
        "rec_gate_b": nrm(ks[15], (N_REC_LAYERS, RNN_HEADS, 2 * RNN_BLOCK), 0.01),
        "rec_a_param": -jnp.log(u ** -0.5 - 1.0),
        "rec_w_out": nrm(ks[17], (N_REC_LAYERS, D_RNN, D_MODEL), D_RNN ** -0.5),
        "moe_w_group": nrm(ks[18], (DEPTH, D_MODEL, N_GROUPS), D_MODEL ** -0.5),
        "moe_b_group": nrm(ks[19], (DEPTH, N_GROUPS), 0.01),
        "moe_w_expert": nrm(ks[20], (DEPTH, D_MODEL, N_EXPERTS), D_MODEL ** -0.5),
        "moe_b_expert": nrm(ks[21], (DEPTH, N_EXPERTS), 0.01),
        "moe_w13": nrm(ks[22], (DEPTH, N_EXPERTS, D_MODEL, 2 * D_EXPERT), D_MODEL ** -0.5),
        "moe_w2": nrm(ks[23], (DEPTH, N_EXPERTS, D_EXPERT, D_MODEL), D_EXPERT ** -0.5),
    }


def reference(x, c, norm_mix, norm_ffn, final_norm, ada_w, ada_b,
              attn_w_qkv, attn_lambda, attn_subln, attn_w_o,
              rec_w_in, rec_conv_w, rec_conv_b, rec_gate_w, rec_gate_b, rec_a_param, rec_w_out,
              moe_w_group, moe_b_group, moe_w_expert, moe_b_expert, moe_w13, moe_w2):
    cond = jax.nn.silu(c)
    for layer in range(DEPTH):
        mod = cond @ ada_w[layer] + ada_b[layer]
        sh1, sc1, g1, sh2, sc2, g2 = jnp.split(mod, 6, axis=-1)
        hm = modulate(rms_norm(x, norm_mix[layer]), sh1, sc1)
        j = layer // N_MIXERS
        if layer % N_MIXERS == 0:
            mix = diff_attention(hm, attn_w_qkv[j], attn_lambda[j], attn_subln[j], attn_w_o[j],
                                 lambda_init_fn(layer))
        else:
            mix = recurrent_block(hm, rec_w_in[j], rec_conv_w[j], rec_conv_b[j], rec_gate_w[j],
                                  rec_gate_b[j], rec_a_param[j], rec_w_out[j])
        x = x + g1[:, None, :] * mix
        hf = modulate(rms_norm(x, norm_ffn[layer]), sh2, sc2)
        x = x + g2[:, None, :] * hier_moe(hf, moe_w_group[layer], moe_b_group[layer], moe_w_expert[layer],
                                          moe_b_expert[layer], moe_w13[layer], moe_w2[layer])
    return rms_norm(x, final_norm)
```

```python
from contextlib import ExitStack
import math
import numpy as np
import concourse.bass as bass
import concourse.mybir as mybir
from concourse.bass_utils import run_bass_kernel_spmd

F32 = mybir.dt.float32
BF16 = mybir.dt.bfloat16
ALU = mybir.AluOpType
AF = mybir.ActivationFunctionType
AX = mybir.AxisListType

D = 1024
KC = 8
NEXP = 16
DE = 512


class StopBuild(Exception):
    pass


class Res:
    __slots__ = ("name", "w", "r")

    def __init__(self, name=""):
        self.name = name
        self.w = {}
        self.r = {}


class DSem:
    def __init__(self, nc, name):
        self.sem = nc.alloc_semaphore(name)
        self.key = name
        self.val = 0


class Sched:
    def __init__(self, nc):
        self.nc = nc
        self.E = {"pe": nc.tensor, "act": nc.scalar, "dve": nc.vector, "pool": nc.gpsimd, "sp": nc.sync}
        self.sem = {e: nc.alloc_semaphore("sem_" + e) for e in ("pe", "act", "dve", "pool")}
        self.cnt = {e: 0 for e in self.sem}
        self.waited = {e: {} for e in self.E}
        self.dsems = []
        self.nd = 0

    def dsem(self, name):
        d = DSem(self.nc, "d_%s_%d" % (name, self.nd))
        self.nd += 1
        self.dsems.append(d)
        return d

    def _wait(self, e, tok):
        key, sem, val, src = tok
        if src == e and e == "pe":
            return
        if self.waited[e].get(key, 0) >= val:
            return
        self.E[e].wait_ge(sem, val)
        self.waited[e][key] = val

    def _deps(self, e, reads, writes):
        for r in reads:
            for t in r.w.values():
                self._wait(e, t)
        for w in writes:
            for t in w.w.values():
                self._wait(e, t)
            for t in w.r.values():
                self._wait(e, t)

    def _commit(self, tok, reads, writes):
        for r in reads:
            r.r[tok[0]] = tok
        for w in writes:
            w.w = {tok[0]: tok}
            w.r = {}

    def op(self, e, fn, reads=(), writes=()):
        self._deps(e, reads, writes)
        ins = fn(self.E[e])
        ins.then_inc(self.sem[e], 1)
        self.cnt[e] += 1
        tok = (e, self.sem[e], self.cnt[e], e)
        self._commit(tok, reads, writes)
        return tok

    def pe_group(self, fns, reads=(), writes=()):
        self._deps("pe", reads, writes)
        ins = None
        for f in fns:
            ins = f(self.E["pe"])
        ins.then_inc(self.sem["pe"], 1)
        self.cnt["pe"] += 1
        tok = ("pe", self.sem["pe"], self.cnt["pe"], "pe")
        self._commit(tok, reads, writes)
        return tok

    def dma(self, q, ds, out, in_, reads=(), writes=()):
        self._deps(q, reads, writes)
        self.E[q].dma_start(out=out, in_=in_).then_inc(ds.sem, 16)
        ds.val += 16
        tok = (ds.key, ds.sem, ds.val, "dma")
        self._commit(tok, reads, writes)
        return tok

    def barrier(self):
        for e in self.E:
            for o in self.sem:
                if o != e and self.cnt[o] > 0:
                    self._wait(e, (o, self.sem[o], self.cnt[o], o))
            for d in self.dsems:
                if d.val > 0:
                    self._wait(e, (d.key, d.sem, d.val, "dma"))


def lambda_init_fn(layer):
    return 0.8 - 0.6 * math.exp(-0.3 * layer)


def build(cfg):
    S_ = cfg["S"]
    NSEQ = cfg["nseq"]
    LAYERS = cfg["layers"]
    NE = cfg.get("ne", NEXP)
    DBG = cfg.get("dbg", {})
    NB = S_ // 512
    NT = S_ // 128
    nc = bass.Bass("TRN2", target_bir_lowering=False)

    def dr(name, shape):
        return nc.dram_tensor(name, list(shape), F32, kind="ExternalInput").ap()

    x = dr("x", [NSEQ, S_, D])
    c_in = dr("c", [NSEQ, D])
    norm_mix = dr("norm_mix", [4, D])
    norm_ffn = dr("norm_ffn", [4, D])
    final_norm = dr("final_norm", [D])
    ada_w = dr("ada_w", [4, D, 6 * D])
    ada_b = dr("ada_b", [4, 6 * D])
    attn_w_qkv = dr("attn_w_qkv", [2, D, 3 * D])
    attn_lambda = dr("attn_lambda", [2, 4, 64])
    attn_subln = dr("attn_subln", [2, 128])
    attn_w_o = dr("attn_w_o", [2, D, D])
    rec_w_in = dr("rec_w_in", [2, D, 2 * D])
    rec_conv_w = dr("rec_conv_w", [2, 4, D])
    rec_conv_b = dr("rec_conv_b", [2, D])
    rec_gate_w = dr("rec_gate_w", [2, 4, 256, 512])
    rec_gate_b = dr("rec_gate_b", [2, 4, 512])
    rec_a_param = dr("rec_a_param", [2, D])
    rec_w_out = dr("rec_w_out", [2, D, D])
    moe_w_group = dr("moe_w_group", [4, D, 4])
    moe_b_group = dr("moe_b_group", [4, 4])
    moe_w_expert = dr("moe_w_expert", [4, D, 16])
    moe_b_expert = dr("moe_b_expert", [4, 16])
    moe_w13 = dr("moe_w13", [4, 16, D, 2 * DE])
    moe_w2 = dr("moe_w2", [4, 16, DE, D])
    out = nc.dram_tensor("out", [NSEQ, S_, D], F32, kind="ExternalOutput").ap()
    dbg_out = {}
    for name, shape in DBG.items():
        dbg_out[name] = nc.dram_tensor("dbg_" + name, list(shape), F32, kind="ExternalOutput").ap()

    S = Sched(nc)
    es = ExitStack()
    STOP = cfg.get("stop", 99)

    def stop_at(n):
        if STOP == n:
            S.barrier()
            raise StopBuild()

    sbn = [0]

    def sb(name, shape, dt=F32, stack=None):
        sbn[0] += 1
        return (stack or es).enter_context(nc.sbuf_tensor("%s_%d" % (name, sbn[0]), list(shape), dt))

    try:
      with es:
        PS = [es.enter_context(nc.psum_tensor("ps%d" % i, [128, 512], F32)) for i in range(8)]
        PSR = [Res("ps%d" % i) for i in range(8)]
        psi = [0]

        ring = {"banks": list(range(8))}

        def set_ring(banks):
            ring["banks"] = list(banks)

        def ps_next():
            bk = ring["banks"]
            i = bk[psi[0] % len(bk)]
            psi[0] += 1
            return PS[i], PSR[i]

        def ps_fixed(i):
            return PS[i], PSR[i]

        ident = sb("ident", [128, 128])
        onesD = sb("onesD", [128, 128], BF16)
        onesH = sb("onesH", [128, 128], BF16)
        ones1 = sb("ones1", [128, 128], BF16)
        ones_row = sb("ones_row", [1, 128])
        selm = sb("selm", [16, NEXP, 128], BF16)
        maskT = sb("maskT", [128, 128], BF16)
        tmp128 = sb("tmp128", [128, 128])
        epsc = sb("epsc", [128, 4])
        xT = sb("xT", [128, KC, S_])
        hT = sb("hT", [128, KC, S_], BF16)
        prmT = sb("prmT", [128, 4, 128])
        modT = sb("modT", [128, 4, 48, NSEQ])
        condT = sb("condT", [128, KC, NSEQ])
        lamv = sb("lamv", [128, 2, 2])
        NSTG = 2
        stg = [sb("stg%d" % i, [128, 2048]) for i in range(NSTG)]
        stg_res = [Res("stg%d" % i) for i in range(NSTG)]
        stg_ds = [S.dsem("stg%d" % i) for i in range(NSTG)]
        stg_i = [0]
        R_const = Res("const")
        R_x = [Res("xT%d" % b) for b in range(NB)]
        R_h = [Res("hT%d" % b) for b in range(NB)]
        R_prm = Res("prm")
        R_mod = Res("mod")
        R_misc = Res("misc")
        ob_ds = [S.dsem("ob") for _ in range(2)]
        xin_ds = [S.dsem("xin") for _ in range(2)]
        rw_ds = S.dsem("rw")
        dbg_ds = S.dsem("dbg")

        def dbg(name, src_ap, res):
            if name in dbg_out:
                S.dma("sp", dbg_ds, dbg_out[name], src_ap, reads=res)

        S.op("pool", lambda g: g.memset(tmp128[:], 1.0), writes=[R_misc])
        S.op("pool", lambda g: g.affine_select(out=ident[:], in_=tmp128[:], pattern=[[1, 128]],
                                                compare_op=ALU.is_equal, fill=0.0, base=0, channel_multiplier=-1),
             reads=[R_misc], writes=[R_const])
        S.op("pool", lambda g: g.affine_select(out=maskT[:], in_=tmp128[:], pattern=[[1, 128]],
                                                compare_op=ALU.is_ge, fill=0.0, base=0, channel_multiplier=-1),
             reads=[R_misc], writes=[R_const])
        S.op("pool", lambda g: g.affine_select(out=selm[:], in_=tmp128[0:16, :].unsqueeze(1).broadcast_to([16, NEXP, 128]),
                                                pattern=[[1, NEXP], [0, 128]],
                                                compare_op=ALU.is_equal, fill=0.0, base=0, channel_multiplier=-1),
             reads=[R_misc], writes=[R_const])
        S.op("dve", lambda v: v.memset(onesD[:], 1.0 / 1024.0), writes=[R_const])
        S.op("dve", lambda v: v.memset(onesH[:], 1.0 / 128.0), writes=[R_const])
        S.op("dve", lambda v: v.memset(ones1[:], 1.0), writes=[R_const])
        S.op("dve", lambda v: v.memset(ones_row[:], 1.0), writes=[R_const])
        S.op("dve", lambda v: v.memset(epsc[:, 0:1], 1e-6), writes=[R_const])
        S.op("dve", lambda v: v.memset(epsc[:, 1:2], 1e-5), writes=[R_const])
        S.op("dve", lambda v: v.memset(epsc[:, 2:3], 1.0), writes=[R_const])
        S.op("dve", lambda v: v.memset(epsc[:, 3:4], 0.0), writes=[R_const])

        stop_at(1)
        adab_rows = ada_b.rearrange("l (r p) -> (l r) p", p=128)
        G2 = {"nm": 0, "nf": 32, "fin": 64, "cb": 72, "ap": 88, "sub": 104}
        G3 = {"cw": 0, "gb": 64}
        with ExitStack() as ps_stack:
            rowbuf = [sb("rowbuf%d" % g, [128, 128], stack=ps_stack) for g in range(4)]
            R_rb = [Res("rb%d" % g) for g in range(4)]
            rb_dsl = [S.dsem("rb") for _ in range(4)]
            rb_ds = S.dsem("rbx")
            l_ds = S.dsem("lrow")
            for g in range(4):
                S.op("pool", lambda gp, g=g: gp.memset(rowbuf[g][:], 0.0), writes=[R_rb[g]])
            S.dma("sp", rb_dsl[0], rowbuf[0][0:128, :], adab_rows[0:128, :], writes=[R_rb[0]])
            S.dma("sp", rb_dsl[1], rowbuf[1][0:64, :], adab_rows[128:192, :], writes=[R_rb[1]])
            S.dma("sp", rb_dsl[2], rowbuf[2][0:32, :], norm_mix.rearrange("l (r p) -> (l r) p", p=128), writes=[R_rb[2]])
            S.dma("sp", rb_dsl[2], rowbuf[2][32:64, :], norm_ffn.rearrange("l (r p) -> (l r) p", p=128), writes=[R_rb[2]])
            S.dma("sp", rb_dsl[2], rowbuf[2][64:72, :], final_norm.rearrange("(r p) -> r p", p=128), writes=[R_rb[2]])
            S.dma("sp", rb_dsl[2], rowbuf[2][72:88, :], rec_conv_b.rearrange("l (r p) -> (l r) p", p=128), writes=[R_rb[2]])
            S.dma("sp", rb_dsl[2], rowbuf[2][88:104, :], rec_a_param.rearrange("l (r p) -> (l r) p", p=128), writes=[R_rb[2]])
            S.dma("sp", rb_dsl[2], rowbuf[2][104:106, :], attn_subln, writes=[R_rb[2]])
            S.dma("sp", rb_dsl[3], rowbuf[3][0:64, :], rec_conv_w.rearrange("l i (r p) -> (l i r) p", p=128), writes=[R_rb[3]])
            S.dma("sp", rb_dsl[3], rowbuf[3][64:96, :], rec_gate_b.rearrange("l h (r p) -> (l h r) p", p=128), writes=[R_rb[3]])
            for g in range(4):
                pt, pr = ps_next()
                S.pe_group([lambda pe, g=g, pt=pt: pe.transpose(out=pt[:, 0:128], in_=rowbuf[g][:], identity=ident[:])],
                           reads=[R_rb[g], R_const], writes=[pr])
                S.op("dve", lambda v, g=g, pt=pt: v.tensor_copy(out=prmT[:, g, :], in_=pt[:, 0:128]),
                     reads=[pr], writes=[R_prm])

            stop_at(2)
            crow = sb("crow", [NSEQ, D], stack=ps_stack)
            R_crow = Res("crow")
            S.dma("sp", rb_ds, crow[:], c_in, writes=[R_crow])
            S.op("act", lambda a: a.activation(out=crow[:], in_=crow[:], func=AF.Silu), reads=[R_crow], writes=[R_crow])
            pt, pr = ps_next()
            S.pe_group([lambda pe, k=k, pt=pt: pe.transpose(out=pt[:, k * NSEQ:(k + 1) * NSEQ],
                                                             in_=crow[:, k * 128:(k + 1) * 128],
                                                             identity=ident[0:NSEQ, 0:NSEQ]) for k in range(KC)],
                       reads=[R_crow, R_const], writes=[pr])
            S.op("dve", lambda v, pt=pt: v.tensor_copy(out=condT[:].rearrange("p k b -> p (k b)"), in_=pt[:, 0:KC * NSEQ]),
                 reads=[pr], writes=[R_mod])

            stop_at(3)
            lrow = sb("lrow", [1, 2, 256], stack=ps_stack)
            lprod = sb("lprod", [1, 2, 128], stack=ps_stack)
            lsum = sb("lsum", [1, 2, 2], stack=ps_stack)
            lres = sb("lres", [1, 2], stack=ps_stack)
            R_l = Res("lam")
            S.dma("sp", l_ds, lrow[:], attn_lambda.rearrange("j a d -> j (a d)").unsqueeze(0), writes=[R_l])
            lr4 = lrow[:].rearrange("o j (a t d) -> o j a t d", a=2, t=2)
            S.op("dve", lambda v: v.tensor_tensor(out=lprod[:].rearrange("o j (a d) -> o j a d", a=2),
                                                  in0=lr4[:, :, :, 0, :], in1=lr4[:, :, :, 1, :], op=ALU.mult),
                 reads=[R_l], writes=[R_l])
            S.op("dve", lambda v: v.tensor_reduce(out=lsum[:], in_=lprod[:].rearrange("o j (a d) -> o j a d", a=2),
                                                  axis=AX.X, op=ALU.add), reads=[R_l], writes=[R_l])
            S.op("act", lambda a: a.activation(out=lsum[:], in_=lsum[:], func=AF.Exp), reads=[R_l], writes=[R_l])
            S.op("dve", lambda v: v.tensor_tensor(out=lres[:], in0=lsum[:, :, 1], in1=lsum[:, :, 0], op=ALU.subtract),
                 reads=[R_l], writes=[R_l])
            for j in range(2):
                S.op("dve", lambda v, j=j: v.tensor_scalar(out=lres[:, j:j + 1], in0=lres[:, j:j + 1],
                                                           scalar1=-lambda_init_fn(2 * j), scalar2=None, op0=ALU.add),
                     reads=[R_l], writes=[R_l])
            pt, pr = ps_next()
            S.pe_group([lambda pe, pt=pt: pe.matmul(pt[:, 0:2], lhsT=ones_row[:], rhs=lres[:], start=True, stop=True)],
                       reads=[R_l, R_const], writes=[pr])
            S.op("dve", lambda v, pt=pt: v.tensor_copy(out=lamv[:, :, 0], in_=pt[:, 0:2]), reads=[pr], writes=[R_prm])

            stop_at(4)
            NPS = 6
            pstg = [sb("pstg%d" % i, [128, 2048], stack=ps_stack) for i in range(NPS)]
            pstg_res = [Res("pstg%d" % i) for i in range(NPS)]
            pstg_ds = [S.dsem("pstg%d" % i) for i in range(NPS)]
            pstg_i = 0
            for l in LAYERS:
                pm, pmr = ps_next()
                for piece in range(24):
                    si = pstg_i % NPS
                    pstg_i += 1
                    src = ada_w[l, :, piece * 256:(piece + 1) * 256].rearrange("(k p) n -> p k n", p=128)
                    S.dma("sp", pstg_ds[si], pstg[si][:].rearrange("p (k n) -> p k n", k=KC), src, writes=[pstg_res[si]])
                    sv = pstg[si][:].rearrange("p (k n) -> p k n", k=KC)
                    for m in range(2):
                        oc = piece * 2 + m
                        S.pe_group([lambda pe, k=k, m=m, oc=oc, sv=sv, pm=pm:
                                    pe.matmul(pm[:, oc * NSEQ:(oc + 1) * NSEQ], lhsT=sv[:, k, m * 128:(m + 1) * 128],
                                              rhs=condT[:, k, :], start=(k == 0), stop=(k == KC - 1))
                                    for k in range(KC)],
                                   reads=[pstg_res[si], R_mod], writes=[pmr] if (piece == 0 and m == 0) else [])
                pmr.w = {"pe": ("pe", S.sem["pe"], S.cnt["pe"], "pe")}
                abcol = prmT[:].rearrange("p g r -> p (g r)")[:, l * 48:(l + 1) * 48]
                S.op("dve", lambda v, l=l, pm=pm, abcol=abcol: v.tensor_tensor(
                    out=modT[:, l, :, :], in0=pm[:, 0:48 * NSEQ].rearrange("p (c b) -> p c b", b=NSEQ),
                    in1=abcol.unsqueeze(2).broadcast_to([128, 48, NSEQ]), op=ALU.add),
                    reads=[pmr, R_prm], writes=[R_mod])
            S.barrier()

        def prm(g, r):
            return prmT[:, g, r:r + 1]

        def modc(l, i, k, b):
            return modT[:, l, i * 8 + k, b:b + 1]

        def load_w(dst_ap, dst_res, srcs, eng):
            si = stg_i[0] % NSTG
            stg_i[0] += 1
            n = 0
            for (lo, hi, shape_fn, src) in srcs:
                S.dma("sp", stg_ds[si], shape_fn(stg[si][:, lo:hi]), src, writes=[stg_res[si]])
                n = max(n, hi)
            if eng == "act":
                S.op("act", lambda a: a.copy(out=dst_ap, in_=stg[si][:, 0:n]), reads=[stg_res[si]], writes=[dst_res])
            else:
                S.op(eng, lambda g: g.tensor_copy(out=dst_ap, in_=stg[si][:, 0:n]), reads=[stg_res[si]], writes=[dst_res])

        cast_rr = [0]

        cast_mode = ["rr"]

        def cast_eng():
            if cast_mode[0] != "rr":
                return cast_mode[0]
            cast_rr[0] += 1
            return "pool" if cast_rr[0] % 2 else "act"

        def load_rows(dst_tile, dst_res, w2d, r0, nrow_chunks, c0, ncols):
            per = max(1, 2048 // ncols)
            k = 0
            while k < nrow_chunks:
                kk = min(per, nrow_chunks - k)
                src = w2d[r0 + k * 128: r0 + (k + kk) * 128, c0:c0 + ncols].rearrange("(k p) n -> p k n", p=128)
                load_w(dst_tile[:, k:k + kk, :].rearrange("p k n -> p (k n)"), dst_res,
                       [(0, kk * ncols, (lambda v, kk=kk: v.rearrange("p (k n) -> p k n", k=kk)), src)], cast_eng())
                k += kk

        def norm_phase(st, gcol, sc_i, sh_i, l, b, router=None):
            with ExitStack() as st:
                _norm_phase(st, gcol, sc_i, sh_i, l, b, router)
                S.barrier()

        def _norm_phase(st, gcol, sc_i, sh_i, l, b, router=None):
            Acol = sb("Acol", [128, KC], stack=st)
            R_A = Res("A")
            for k in range(KC):
                S.op("dve", lambda v, k=k: v.scalar_tensor_tensor(out=Acol[:, k:k + 1], in0=modc(l, sc_i, k, b),
                                                                  scalar=gcol(k), in1=gcol(k), op0=ALU.mult, op1=ALU.add),
                     reads=[R_mod, R_prm], writes=[R_A])
            sq1 = sb("sq", [128, KC, 512], BF16, stack=st)
            sq = [sq1, sq1]
            R_sq1 = Res("sq")
            R_sq = [R_sq1, R_sq1]
            rstd = [sb("rstd%d" % i, [128, 512], stack=st) for i in range(2)]
            R_rstd = [Res("rstd") for _ in range(2)]
            NTM = 2
            tmp = [sb("ntmp%d" % i, [128, 512], stack=st) for i in range(NTM)]
            R_tmp = [Res("ntmp") for _ in range(NTM)]
            ti = 0
            if router is not None:
                hf32 = [sb("hf32_%d" % i, [128, 512], stack=st) for i in range(NTM)]
                R_hf = [Res("hf32") for _ in range(NTM)]
            for blk in range(NB):
                bs = slice(blk * 512, (blk + 1) * 512)
                i2 = blk % 2
                S.op("act", lambda a, bs=bs, i2=i2: a.activation(out=sq[i2][:], in_=xT[:, :, bs], func=AF.Square),
                     reads=[R_x[blk]], writes=[R_sq[i2]])
                pt, pr = ps_next()
                S.pe_group([lambda pe, k=k, pt=pt, i2=i2: pe.matmul(pt[:], lhsT=onesD[:], rhs=sq[i2][:, k, :],
                                                                     start=(k == 0), stop=(k == KC - 1)) for k in range(KC)],
                           reads=[R_sq[i2], R_const], writes=[pr])
                S.op("act", lambda a, pt=pt, i2=i2: a.activation(out=rstd[i2][:], in_=pt[:], func=AF.Ln, bias=epsc[:, 0:1], scale=1.0),
                     reads=[pr, R_const], writes=[R_rstd[i2]])
                S.op("act", lambda a, i2=i2: a.activation(out=rstd[i2][:], in_=rstd[i2][:], func=AF.Exp, scale=-0.5), reads=[R_rstd[i2]], writes=[R_rstd[i2]])
                if router is not None:
                    pl, plr = ps_next()
                for k in range(KC):
                    t3 = ti % NTM
                    ti += 1
                    S.op("dve", lambda v, k=k, bs=bs, t3=t3, i2=i2: v.tensor_tensor(out=tmp[t3][:], in0=xT[:, k, bs],
                                                                                    in1=rstd[i2][:], op=ALU.mult),
                         reads=[R_x[blk], R_rstd[i2]], writes=[R_tmp[t3]])
                    if router is None:
                        S.op("act", lambda a, k=k, bs=bs, t3=t3: a.activation(out=hT[:, k, bs], in_=tmp[t3][:], func=AF.Identity,
                                                                              bias=modc(l, sh_i, k, b), scale=Acol[:, k:k + 1]),
                             reads=[R_tmp[t3], R_A, R_mod], writes=[R_h[blk]])
                    else:
                        S.op("act", lambda a, k=k, t3=t3: a.activation(out=hf32[t3][:], in_=tmp[t3][:], func=AF.Identity,
                                                                       bias=modc(l, sh_i, k, b), scale=Acol[:, k:k + 1]),
                             reads=[R_tmp[t3], R_A, R_mod], writes=[R_hf[t3]])
                        S.op("pool", lambda g, k=k, bs=bs, t3=t3: g.tensor_copy(out=hT[:, k, bs], in_=hf32[t3][:]),
                             reads=[R_hf[t3]], writes=[R_h[blk]])
                        S.pe_group([lambda pe, k=k, t3=t3, pl=pl: pe.matmul(pl[0:20, :], lhsT=router["w"][:, k, :], rhs=hf32[t3][:],
                                                                             start=(k == 0), stop=(k == KC - 1))],
                                   reads=[R_hf[t3], router["wres"]], writes=[plr] if k == 0 else [])
                if router is not None:
                    plr.w = {"pe": ("pe", S.sem["pe"], S.cnt["pe"], "pe")}
                    S.op("act", lambda a, pl=pl, bs=bs: a.activation(out=router["lgT"][:, bs], in_=pl[0:20, :], func=AF.Identity,
                                                                     bias=router["bias"][:, 0:1], scale=1.0),
                         reads=[plr, router["wres"]], writes=[router["lgres"]])

        def resid_add(pt, pr, gi, l, b, m, blk):
            bs = slice(blk * 512, (blk + 1) * 512)
            S.op("dve", lambda v: v.scalar_tensor_tensor(out=xT[:, m, bs], in0=pt[:], scalar=modc(l, gi, m, b), in1=xT[:, m, bs],
                                                         op0=ALU.mult, op1=ALU.add),
                 reads=[pr, R_mod], writes=[R_x[blk]])

        def attn_phase(l, b):
            j = l // 2
            wq = attn_w_qkv[j]
            with ExitStack() as st:
                norm_phase(st, lambda k: prm(2, G2["nm"] + l * 8 + k), 1, 0, l, b)
                Gs = sb("Gs", [128, 1], stack=st)
                R_G = Res("G")
                S.op("dve", lambda v: v.tensor_scalar(out=Gs[:], in0=prm(2, G2["sub"] + j), scalar1=1.0 - lambda_init_fn(l),
                                                      scalar2=None, op0=ALU.mult), reads=[R_prm], writes=[R_G])
                wqkv = [sb("wqkv%d" % i, [128, 3, KC, 128], BF16, stack=st) for i in range(2)]
                R_wqkv = [Res("wqkv") for _ in range(2)]
                wo = [sb("wo%d" % i, [128, 1, D], BF16, stack=st) for i in range(2)]
                R_wo = [Res("wo") for _ in range(2)]
                qT = [sb("qT%d" % i, [128, S_], BF16, stack=st) for i in range(2)]
                kT = [sb("kT%d" % i, [128, 2, S_], BF16, stack=st) for i in range(2)]
                vh = [sb("vh%d" % i, [128, NT, 128], BF16, stack=st) for i in range(2)]
                oh = [sb("oh%d" % i, [128, S_], BF16, stack=st) for i in range(2)]
                R_q = [Res("q") for _ in range(2)]
                R_k = [Res("k") for _ in range(2)]
                for i in range(2):
                    S.op("pool", lambda g, i=i: g.memset(kT[i][:], 0.0), writes=[R_k[i]])
                R_v = [Res("v") for _ in range(2)]
                R_o = [Res("o") for _ in range(2)]
                NET = 6
                et = [sb("et%d" % i, [128, 512], BF16, stack=st) for i in range(NET)]
                R_et = [Res("et") for _ in range(NET)]
                eti = 0
                sring = [0]
                on = [sb("on%d" % i, [128, 512], stack=st) for i in range(2)]
                R_on = [Res("on") for _ in range(2)]
                rz = sb("rz", [128, 512], stack=st)
                R_rz = Res("rz")
                od = sb("od", [128, 512], stack=st)
                R_od = Res("od")
                osq = sb("osq", [128, 512], BF16, stack=st)
                R_osq = Res("osq")
                orstd = sb("orstd", [128, 512], stack=st)
                R_orstd = Res("orstd")
                set_ring([0, 1, 2, 3, 7])
                pend2 = []
                for h in range(8):
                    hb = h % 2
                    if h == 0:
                        for which in range(3):
                            load_rows(wqkv[hb][:, which, :, :], R_wqkv[hb], wq, 0, KC, which * D + h * 128, 128)
                    load_rows(wo[hb], R_wo[hb], attn_w_o[j], h * 128, 1, 0, D)
                    for blk in range(NB):
                        bs = slice(blk * 512, (blk + 1) * 512)
                        for which, dst, dres in ((0, qT, R_q), (1, kT, R_k)):
                            pt, pr = ps_next()
                            S.pe_group([lambda pe, k=k, pt=pt, which=which, bs=bs: pe.matmul(
                                pt[:], lhsT=wqkv[hb][:, which, k, :], rhs=hT[:, k, bs], start=(k == 0), stop=(k == KC - 1))
                                for k in range(KC)], reads=[R_wqkv[hb], R_h[blk]], writes=[pr])
                            if which == 0:
                                S.op("dve", lambda v, pt=pt, bs=bs: v.tensor_scalar(out=qT[hb][:, bs], in0=pt[:], scalar1=0.125, scalar2=None, op0=ALU.mult),
                                     reads=[pr], writes=[dres[hb]])
                            else:
                                S.op("dve", lambda v, pt=pt, bs=bs: v.tensor_copy(out=kT[hb][0:64, 0, bs], in_=pt[0:64, :]),
                                     reads=[pr], writes=[dres[hb]])
                                S.op("dve", lambda v, pt=pt, bs=bs: v.tensor_copy(out=kT[hb][64:128, 1, bs], in_=pt[64:128, :]),
                                     reads=[pr], writes=[dres[hb]])
                    for tg in range(NT // 4):
                        pt, pr = ps_next()
                        fns = []
                        for tt in range(4):
                            t = tg * 4 + tt
                            for k in range(KC):
                                fns.append(lambda pe, k=k, t=t, tt=tt, pt=pt: pe.matmul(
                                    pt[:, tt * 128:(tt + 1) * 128], lhsT=hT[:, k, t * 128:(t + 1) * 128], rhs=wqkv[hb][:, 2, k, :],
                                    start=(k == 0), stop=(k == KC - 1)))
                        S.pe_group(fns, reads=[R_wqkv[hb], R_h[(tg * 4 * 128) // 512]], writes=[pr])
                        S.op("dve", lambda v, pt=pt, tg=tg: v.tensor_copy(out=vh[hb][:, tg * 4:(tg + 1) * 4, :].rearrange("p t f -> p (t f)"),
                                                                          in_=pt[:]), reads=[pr], writes=[R_v[hb]])
                    set_ring([7])
                    steps = [(qb, n, kt) for qb in range(NB) for n in range(2) for kt in range(4 * qb + 4)]
                    sps = {}

                    def emit_S(i):
                        qb, n, kt = steps[i]
                        c0 = max(0, kt - 4 * qb) * 128
                        bi = 4 + (sring[0] % 4)
                        sring[0] += 1
                        pss, r_s = ps_fixed(bi)
                        S.pe_group([lambda pe: pe.matmul(
                            pss[:, c0:512], lhsT=kT[hb][:, n, kt * 128:(kt + 1) * 128],
                            rhs=qT[hb][:, qb * 512 + c0:(qb + 1) * 512], start=True, stop=True)],
                            reads=[R_q[hb], R_k[hb]], writes=[r_s])
                        sps[i] = (pss, r_s, c0)

                    while pend2:
                        pend2.pop(0)()
                    if h + 1 < 8:
                        for which in range(3):
                            load_rows(wqkv[1 - hb][:, which, :, :], R_wqkv[1 - hb], wq, 0, KC, which * D + (h + 1) * 128, 128)
                    emit_S(0)
                    emit_S(1)
                    emit_S(2)
                    pend1 = []

                    def part1(qb, n):
                        ps_o, r_o = ps_fixed(2 * n)
                        ps_z, r_z = ps_fixed(2 * n + 1)
                        S.op("act", lambda a: a.activation(out=rz[:], in_=ps_z[:], func=AF.Ln), reads=[r_z], writes=[R_rz])
                        S.op("act", lambda a: a.activation(out=rz[:], in_=rz[:], func=AF.Exp, scale=-1.0), reads=[R_rz], writes=[R_rz])
                        S.op("dve", lambda v: v.tensor_tensor(out=on[n][:], in0=ps_o[:], in1=rz[:], op=ALU.mult),
                             reads=[r_o, R_rz], writes=[R_on[n]])
                        if n == 0:
                            return
                        S.op("dve", lambda v: v.scalar_tensor_tensor(out=od[:], in0=on[1][:], scalar=lamv[:, j, 0:1], in1=on[0][:],
                                                                     op0=ALU.mult, op1=ALU.add),
                             reads=[R_on[0], R_on[1], R_prm], writes=[R_od])
                        S.op("pool", lambda g: g.tensor_tensor(out=osq[:], in0=od[:], in1=od[:], op=ALU.mult), reads=[R_od], writes=[R_osq])

                    def part2(qb, hb=hb):
                        pt, pr = ps_fixed(3)
                        S.pe_group([lambda pe: pe.matmul(pt[:], lhsT=onesH[:], rhs=osq[:], start=True, stop=True)],
                                   reads=[R_osq, R_const], writes=[pr])
                        S.op("act", lambda a: a.activation(out=orstd[:], in_=pt[:], func=AF.Ln, bias=epsc[:, 1:2], scale=1.0),
                             reads=[pr, R_const], writes=[R_orstd])
                        S.op("act", lambda a: a.activation(out=orstd[:], in_=orstd[:], func=AF.Exp, scale=-0.5), reads=[R_orstd], writes=[R_orstd])
                        S.op("dve", lambda v: v.scalar_tensor_tensor(out=oh[hb][:, qb * 512:(qb + 1) * 512], in0=od[:], scalar=Gs[:, 0:1],
                                                                     in1=orstd[:], op0=ALU.mult, op1=ALU.mult),
                             reads=[R_od, R_orstd, R_G], writes=[R_o[hb]])

                    for i, (qb, n, kt) in enumerate(steps):
                        while pend1 and pend1[0][0] <= i:
                            pend1.pop(0)[1]()
                        if i + 3 < len(steps):
                            emit_S(i + 3)
                        pss, r_s, c0 = sps.pop(i)
                        nkt = 4 * qb + 4
                        ps_o, r_o = ps_fixed(2 * n)
                        ps_z, r_z = ps_fixed(2 * n + 1)
                        e4 = eti % NET
                        eti += 1
                        S.op("act", lambda a, pss=pss, c0=c0, e4=e4: a.activation(out=et[e4][:, c0:512], in_=pss[:, c0:512], func=AF.Exp),
                             reads=[r_s], writes=[R_et[e4]])
                        if kt >= 4 * qb:
                            S.op("pool", lambda g, c0=c0, e4=e4: g.tensor_tensor(out=et[e4][:, c0:c0 + 128], in0=et[e4][:, c0:c0 + 128],
                                                                                in1=maskT[:], op=ALU.mult),
                                 reads=[R_const], writes=[R_et[e4]])
                        S.pe_group([lambda pe, kt=kt, c0=c0, e4=e4, ps_o=ps_o, nkt=nkt: pe.matmul(
                            ps_o[:, c0:512], lhsT=vh[hb][:, kt, :], rhs=et[e4][:, c0:512], start=(kt == 0), stop=(kt == nkt - 1)),
                            lambda pe, kt=kt, c0=c0, e4=e4, ps_z=ps_z, nkt=nkt: pe.matmul(
                            ps_z[:, c0:512], lhsT=ones1[:], rhs=et[e4][:, c0:512], start=(kt == 0), stop=(kt == nkt - 1))],
                            reads=[R_et[e4], R_v[hb], R_const], writes=[r_o, r_z] if kt == 0 else [])
                        if kt != nkt - 1:
                            continue
                        tokpe = ("pe", S.sem["pe"], S.cnt["pe"], "pe")
                        r_o.w = {"pe": tokpe}
                        r_z.w = {"pe": tokpe}
                        pend1.append((i + 3, lambda qb=qb, n=n: part1(qb, n)))
                        if n == 1:
                            pend1.append((i + min(4 * qb + 8, 12), lambda qb=qb: part2(qb)))
                    for due, f in pend1:
                        f()
                    pend1 = []
                    set_ring([0, 1, 2, 3, 7])
                    if h % 2 == 1:
                        for blk in range(NB):
                            bs = slice(blk * 512, (blk + 1) * 512)
                            for m in range(KC):
                                pt, pr = ps_next()
                                S.pe_group([lambda pe, pt=pt, m=m, bs=bs, hh=hh: pe.matmul(pt[:], lhsT=wo[hh][:, 0, m * 128:(m + 1) * 128],
                                                                                           rhs=oh[hh][:, bs], start=(hh == 0), stop=(hh == 1))
                                            for hh in range(2)],
                                           reads=[R_wo[0], R_wo[1], R_o[0], R_o[1]], writes=[pr])
                                resid_add(pt, pr, 2, l, b, m, blk)
                set_ring(range(8))
                S.barrier()

        def rec_phase(l, b):
            j = l // 2
            with ExitStack() as st:
                norm_phase(st, lambda k: prm(2, G2["nm"] + l * 8 + k), 1, 0, l, b)
                nsp = sb("nsp", [128, KC, 2], stack=st)
                R_nsp = Res("nsp")
                apc = prmT[:, 2, G2["ap"] + j * 8: G2["ap"] + j * 8 + 8]
                S.op("act", lambda a: a.activation(out=nsp[:, :, 0], in_=apc, func=AF.Exp, scale=-1.0), reads=[R_prm], writes=[R_nsp])
                S.op("act", lambda a: a.activation(out=nsp[:, :, 0], in_=nsp[:, :, 0], func=AF.Ln, bias=epsc[:, 2:3], scale=1.0),
                     reads=[R_nsp], writes=[R_nsp])
                S.op("dve", lambda v: v.tensor_scalar(out=nsp[:, :, 1], in0=nsp[:, :, 0], scalar1=-16.0, scalar2=None, op0=ALU.mult),
                     reads=[R_nsp], writes=[R_nsp])
                S.op("dve", lambda v: v.tensor_scalar(out=nsp[:, :, 0], in0=nsp[:, :, 0], scalar1=-8.0, scalar2=None, op0=ALU.mult),
                     reads=[R_nsp], writes=[R_nsp])
                mb1 = sb("mb", [128, 2, 512], BF16, stack=st)
                R_mb1 = Res("mb")
                wout = [sb("wout%d" % i, [128, 2, D], BF16, stack=st) for i in range(2)]
                R_wout = [Res("wout") for _ in range(2)]
                wy = [sb("wy%d" % i, [128, KC, 256], BF16, stack=st) for i in range(2)]
                wx = [sb("wx%d" % i, [128, KC, 256], BF16, stack=st) for i in range(2)]
                wg = [sb("wg%d" % i, [128, 2, 512], BF16, stack=st) for i in range(2)]
                R_wy = [Res("wy") for _ in range(2)]
                R_wx = [Res("wx") for _ in range(2)]
                R_wg = [Res("wg") for _ in range(2)]
                xrb = [sb("xrb%d" % i, [128, 2, 3 + 512], stack=st) for i in range(2)]
                R_xrb = [Res("xrb") for _ in range(2)]
                cv = [[sb("cv%d_%d" % (jj, i), [128, 512], stack=st) for i in range(2)] for jj in range(2)]
                R_cv = [[Res("cv") for i in range(2)] for jj in range(2)]
                a2b = [sb("a2b%d" % jj, [128, 512], stack=st) for jj in range(2)]
                R_a2 = [Res("a2") for jj in range(2)]
                atb = [[sb("at%d_%d" % (p, jj), [128, 512], stack=st) for jj in range(2)] for p in range(2)]
                itb = [[sb("it%d_%d" % (p, jj), [128, 512], stack=st) for jj in range(2)] for p in range(2)]
                R_at = [[Res("at") for jj in range(2)] for p in range(2)]
                R_it = [[Res("it") for jj in range(2)] for p in range(2)]
                gyb = [sb("gy%d" % jj, [128, 512], stack=st) for jj in range(2)]
                R_gy = [Res("gy") for jj in range(2)]
                xcb1 = sb("xcb", [128, 2, 512], BF16, stack=st)
                R_xcb1 = Res("xcb")
                hsb = [sb("hs%d" % i, [128, 2, 512], stack=st) for i in range(2)]
                R_hs = [Res("hs") for _ in range(2)]

                def stage_A(idx, h, blk):
                    hb = h % 2
                    par = idx % 2
                    if blk == 0:
                        load_rows(wx[hb], R_wx[hb], rec_w_in[j], 0, KC, D + h * 256, 256)
                        load_rows(wg[hb], R_wg[hb], rec_gate_w[j, h], 0, 2, 0, 512)
                    bs = slice(blk * 512, (blk + 1) * 512)
                    xb = blk % 2
                    if blk == 0:
                        S.op("pool", lambda g: g.memset(xrb[xb][:, :, 0:3], 0.0), writes=[R_xrb[xb]])
                    else:
                        S.op("pool", lambda g: g.tensor_copy(out=xrb[xb][:, :, 0:3], in_=xrb[1 - xb][:, :, 512:515]),
                             reads=[R_xrb[1 - xb]], writes=[R_xrb[xb]])
                    for jj in range(2):
                        pt, pr = ps_next()
                        S.pe_group([lambda pe, k=k, pt=pt, jj=jj: pe.matmul(pt[:], lhsT=wx[hb][:, k, jj * 128:(jj + 1) * 128],
                                                                             rhs=hT[:, k, bs], start=(k == 0), stop=(k == KC - 1))
                                    for k in range(KC)], reads=[R_wx[hb], R_h[blk]], writes=[pr])
                        S.op("act", lambda a, pt=pt, jj=jj: a.copy(out=xrb[xb][:, jj, 3:515], in_=pt[:]), reads=[pr], writes=[R_xrb[xb]])
                    xc = []
                    for jj in range(2):
                        cc = 2 * h + jj
                        cw = lambda i, cc=cc: prm(3, G3["cw"] + j * 32 + i * 8 + cc)
                        t0, r0 = cv[jj][0], R_cv[jj][0]
                        S.op("dve", lambda v, t0=t0, jj=jj, cw=cw, cc=cc: v.tensor_scalar(
                            out=t0[:], in0=xrb[xb][:, jj, 0:512], scalar1=cw(0), scalar2=prm(2, G2["cb"] + j * 8 + cc),
                            op0=ALU.mult, op1=ALU.add), reads=[R_xrb[xb], R_prm], writes=[r0])
                        for i in range(1, 4):
                            t1, r1 = cv[jj][i % 2], R_cv[jj][i % 2]
                            S.op("dve", lambda v, t0=t0, t1=t1, i=i, jj=jj, cw=cw: v.scalar_tensor_tensor(
                                out=t1[:], in0=xrb[xb][:, jj, i:i + 512], scalar=cw(i), in1=t0[:], op0=ALU.mult, op1=ALU.add),
                                reads=[R_xrb[xb], R_prm, r0], writes=[r1])
                            t0, r0 = t1, r1
                        xc.append((t0, r0))
                        S.op("pool", lambda g, t0=t0, jj=jj: g.tensor_copy(out=xcb1[:, jj, :], in_=t0[:]),
                             reads=[r0], writes=[R_xcb1])
                    for q in range(4):
                        pt, pr = ps_next()
                        S.pe_group([lambda pe, kk=kk, pt=pt, q=q: pe.matmul(pt[:], lhsT=wg[hb][:, kk, q * 128:(q + 1) * 128],
                                                                             rhs=xcb1[:, kk, :], start=(kk == 0), stop=(kk == 1))
                                    for kk in range(2)], reads=[R_wg[hb], R_xcb1], writes=[pr])
                        gt, gr = (atb[par][q], R_at[par][q]) if q < 2 else (itb[par][q - 2], R_it[par][q - 2])
                        S.op("act", lambda a, pt=pt, gt=gt, q=q: a.activation(out=gt[:], in_=pt[:], func=AF.Sigmoid,
                                                                               bias=prm(3, G3["gb"] + j * 16 + h * 4 + q), scale=1.0),
                             reads=[pr, R_prm], writes=[gr])
                    for jj in range(2):
                        cc = 2 * h + jj
                        at, ar = atb[par][jj], R_at[par][jj]
                        it, ir = itb[par][jj], R_it[par][jj]
                        a2t, a2r = a2b[jj], R_a2[jj]
                        xct, xcr = xc[jj]
                        S.op("act", lambda a, a2t=a2t, at=at, cc=cc: a.activation(out=a2t[:], in_=at[:], func=AF.Exp, scale=nsp[:, cc, 1:2]),
                             reads=[ar, R_nsp], writes=[a2r])
                        S.op("act", lambda a, at=at, cc=cc: a.activation(out=at[:], in_=at[:], func=AF.Exp, scale=nsp[:, cc, 0:1]),
                             reads=[ar, R_nsp], writes=[ar])
                    for jj in range(2):
                        it, ir = itb[par][jj], R_it[par][jj]
                        a2t, a2r = a2b[jj], R_a2[jj]
                        xct, xcr = xc[jj]
                        S.op("act", lambda a, a2t=a2t: a.activation(out=a2t[:], in_=a2t[:], func=AF.Sqrt, bias=epsc[:, 2:3], scale=-1.0),
                             reads=[a2r, R_const], writes=[a2r])
                        S.op("pool", lambda g, it=it, xct=xct: g.tensor_tensor(out=it[:], in0=it[:], in1=xct[:], op=ALU.mult),
                             reads=[ir, xcr], writes=[ir])
                        S.op("dve", lambda v, it=it, a2t=a2t: v.tensor_tensor(out=it[:], in0=it[:], in1=a2t[:], op=ALU.mult),
                             reads=[ir, a2r], writes=[ir])

                def stage_B(idx, h, blk):
                    hb = h % 2
                    par = idx % 2
                    if blk == 0:
                        load_rows(wy[hb], R_wy[hb], rec_w_in[j], 0, KC, h * 256, 256)
                        load_rows(wout[hb], R_wout[hb], rec_w_out[j], h * 256, 2, 0, D)
                    bs = slice(blk * 512, (blk + 1) * 512)
                    xb = blk % 2
                    for jj in range(2):
                        pt, pr = ps_next()
                        S.pe_group([lambda pe, k=k, pt=pt, jj=jj: pe.matmul(pt[:], lhsT=wy[hb][:, k, jj * 128:(jj + 1) * 128],
                                                                             rhs=hT[:, k, bs], start=(k == 0), stop=(k == KC - 1))
                                    for k in range(KC)], reads=[R_wy[hb], R_h[blk]], writes=[pr])
                        S.op("act", lambda a, pt=pt, jj=jj: a.activation(out=gyb[jj][:], in_=pt[:], func=AF.Gelu_apprx_tanh), reads=[pr], writes=[R_gy[jj]])
                    for jj in range(2):
                        init = 0.0 if blk == 0 else hsb[1 - xb][:, jj, 511:512]
                        S.op("dve", lambda v, jj=jj, init=init: v.tensor_tensor_scan(
                            out=hsb[xb][:, jj, :], data0=atb[par][jj][:], data1=itb[par][jj][:], initial=init, op0=ALU.mult, op1=ALU.add),
                            reads=[R_at[par][jj], R_it[par][jj], R_hs[1 - xb]], writes=[R_hs[xb]])
                        S.op("dve", lambda v, jj=jj: v.tensor_tensor(out=mb1[:, jj, :], in0=gyb[jj][:], in1=hsb[xb][:, jj, :], op=ALU.mult),
                             reads=[R_gy[jj], R_hs[xb]], writes=[R_mb1])
                    for m in range(KC):
                        pt, pr = ps_next()
                        S.pe_group([lambda pe, kk=kk, pt=pt, m=m: pe.matmul(pt[:], lhsT=wout[hb][:, kk, m * 128:(m + 1) * 128], rhs=mb1[:, kk, :],
                                                                             start=(kk == 0), stop=(kk == 1)) for kk in range(2)],
                                   reads=[R_wout[hb], R_mb1], writes=[pr])
                        resid_add(pt, pr, 2, l, b, m, blk)

                items = [(h, blk) for h in range(4) for blk in range(NB)]
                stage_A(0, *items[0])
                for i in range(len(items)):
                    if i + 1 < len(items):
                        stage_A(i + 1, *items[i + 1])
                    stage_B(i, *items[i])
                S.barrier()

        def moe_phase(l, b):
            with ExitStack() as st:
                lg = sb("lg", [128, NT, 20], stack=st)
                wexp = [sb("wexp%d" % i, [128, 12, D], BF16, stack=st) for i in range(2)]
                R_wexp = [Res("wexp") for _ in range(2)]
                cast_mode[0] = "act"

                def wpieces(e):
                    eb = e % 2
                    pcs = []
                    for p4 in range(4):
                        pcs.append(lambda p4=p4: load_rows(wexp[eb][:, 2 * p4:2 * p4 + 2, :], R_wexp[eb], moe_w13[l, e], p4 * 256, 2, 0, D))
                    for p2 in range(2):
                        pcs.append(lambda p2=p2: load_rows(wexp[eb][:, 8 + 2 * p2:10 + 2 * p2, :], R_wexp[eb], moe_w2[l, e], p2 * 256, 2, 0, D))
                    return pcs

                for f in wpieces(0):
                    f()
                st1 = ExitStack()
                rw = sb("rw", [128, KC, 20], stack=st1)
                rbias = sb("rbias", [20, 1], stack=st1)
                lgT = sb("lgT", [20, S_], stack=st1)
                R_rw = Res("rw")
                R_lg = Res("lg")
                rds = rw_ds
                with nc.allow_non_contiguous_dma(reason="tiny router weights"):
                    S.dma("sp", rds, rw[:, :, 0:4], moe_w_group[l].rearrange("(k p) n -> p k n", p=128), writes=[R_rw])
                    S.dma("sp", rds, rw[:, :, 4:20], moe_w_expert[l].rearrange("(k p) n -> p k n", p=128), writes=[R_rw])
                    S.dma("sp", rds, rbias[0:4, :], moe_b_group[l].unsqueeze(1), writes=[R_rw])
                    S.dma("sp", rds, rbias[4:20, :], moe_b_expert[l].unsqueeze(1), writes=[R_rw])
                norm_phase(st, lambda k: prm(2, G2["nf"] + l * 8 + k), 4, 3, l, b,
                           router={"w": rw, "wres": R_rw, "bias": rbias, "lgT": lgT, "lgres": R_lg})
                R_lgt = Res("lgt")
                for tg in range(NT // 4):
                    pt, pr = ps_next()
                    S.pe_group([lambda pe, tt=tt, tg=tg, pt=pt: pe.transpose(out=pt[:, tt * 20:(tt + 1) * 20],
                                                                               in_=lgT[:, (tg * 4 + tt) * 128:(tg * 4 + tt + 1) * 128],
                                                                               identity=ident[0:20, 0:20]) for tt in range(4)],
                               reads=[R_lg, R_const], writes=[pr])
                    S.op("dve", lambda v, pt=pt, tg=tg: v.tensor_copy(out=lg[:, tg * 4:(tg + 1) * 4, :].rearrange("p t n -> p (t n)"), in_=pt[:, 0:80]),
                         reads=[pr], writes=[R_lgt])
                S.barrier()
                st1.close()
                gmax = sb("gmax", [128, NT], stack=st)
                goh = sb("goh", [128, NT, 4], stack=st)
                gex = sb("gex", [128, NT, 4], stack=st)
                gsum = sb("gsum", [128, NT], stack=st)
                m16 = sb("m16", [128, NT, 4, 4], stack=st)
                m16b = sb("m16b", [128, NT, 16], stack=st)
                top1 = sb("top1", [128, NT], stack=st)
                top2 = sb("top2", [128, NT], stack=st)
                eq1 = sb("eq1", [128, NT, 16], stack=st)
                den = sb("den", [128, NT], stack=st)
                comb = sb("comb", [128, NT, 16], stack=st)
                R_rt = Res("rt")
                RW = dict(reads=[R_lgt, R_rt], writes=[R_rt])
                gl = lg[:, :, 0:4]
                el = lg[:, :, 4:20].rearrange("p t (g e) -> p t g e", g=4)
                m16f = m16[:].rearrange("p t g e -> p t (g e)")

                def bc(ap2, n):
                    return ap2.unsqueeze(2).broadcast_to([128, NT, n])
                S.op("dve", lambda v: v.tensor_reduce(out=gmax[:], in_=gl, axis=AX.X, op=ALU.max), **RW)
                S.op("dve", lambda v: v.tensor_tensor(out=goh[:], in0=gl, in1=bc(gmax[:], 4), op=ALU.is_equal), **RW)
                S.op("dve", lambda v: v.tensor_tensor(out=gex[:], in0=gl, in1=bc(gmax[:], 4), op=ALU.subtract), **RW)
                S.op("act", lambda a: a.activation(out=gex[:], in_=gex[:], func=AF.Exp), **RW)
                S.op("dve", lambda v: v.tensor_reduce(out=gsum[:], in_=gex[:], axis=AX.X, op=ALU.add), **RW)
                S.op("dve", lambda v: v.tensor_scalar(out=goh[:], in0=goh[:], scalar1=-1.0, scalar2=1e9, op0=ALU.add, op1=ALU.mult), **RW)
                S.op("dve", lambda v: v.tensor_tensor(out=m16[:], in0=el, in1=goh[:].unsqueeze(3).broadcast_to([128, NT, 4, 4]), op=ALU.add), **RW)
                S.op("dve", lambda v: v.tensor_reduce(out=top1[:], in_=m16f, axis=AX.X, op=ALU.max), **RW)
                S.op("dve", lambda v: v.tensor_tensor(out=eq1[:], in0=m16f, in1=bc(top1[:], 16), op=ALU.is_equal), **RW)
                S.op("dve", lambda v: v.scalar_tensor_tensor(out=m16b[:], in0=eq1[:], scalar=-1e9, in1=m16f, op0=ALU.mult, op1=ALU.add), **RW)
                S.op("dve", lambda v: v.tensor_reduce(out=top2[:], in_=m16b[:], axis=AX.X, op=ALU.max), **RW)
                S.op("dve", lambda v: v.tensor_tensor(out=eq1[:], in0=m16f, in1=bc(top2[:], 16), op=ALU.is_ge), **RW)
                S.op("dve", lambda v: v.tensor_tensor(out=m16b[:], in0=m16f, in1=bc(top1[:], 16), op=ALU.subtract), **RW)
                S.op("act", lambda a: a.activation(out=m16b[:], in_=m16b[:], func=AF.Exp), **RW)
                S.op("dve", lambda v: v.tensor_tensor(out=den[:], in0=top2[:], in1=top1[:], op=ALU.subtract), **RW)
                S.op("act", lambda a: a.activation(out=den[:], in_=den[:], func=AF.Exp), **RW)
                S.op("dve", lambda v: v.scalar_tensor_tensor(out=den[:], in0=den[:], scalar=1.0, in1=gsum[:], op0=ALU.add, op1=ALU.mult), **RW)
                S.op("dve", lambda v: v.reciprocal(out=den[:], in_=den[:]), **RW)
                S.op("dve", lambda v: v.tensor_tensor(out=comb[:], in0=eq1[:], in1=m16b[:], op=ALU.mult), **RW)
                S.op("dve", lambda v: v.tensor_tensor(out=comb[:], in0=comb[:], in1=bc(den[:], 16), op=ALU.mult), **RW)
                dbg("comb", comb[:], [R_rt])
                combT = sb("combT", [16, S_], BF16, stack=st)
                R_ct = Res("ct")
                for tg in range(NT // 4):
                    pt, pr = ps_next()
                    S.pe_group([lambda pe, tt=tt, tg=tg, pt=pt: pe.transpose(out=pt[0:16, tt * 128:(tt + 1) * 128], in_=comb[:, tg * 4 + tt, :],
                                                                               identity=ident[:]) for tt in range(4)],
                               reads=[R_rt, R_const], writes=[pr])
                    S.op("dve", lambda v, pt=pt, tg=tg: v.tensor_copy(out=combT[:, tg * 512:(tg + 1) * 512], in_=pt[0:16, :]),
                         reads=[pr], writes=[R_ct])
                actb = [sb("actb%d" % i, [128, 4, 512], BF16, stack=st) for i in range(2)]
                R_actb = [Res("actb") for _ in range(2)]
                NSS = 4
                sbuf_s = [sb("ss%d" % i, [128, 512], stack=st) for i in range(NSS)]
                R_ss = [Res("ss") for _ in range(NSS)]
                csb = [sb("csb%d" % i, [128, 512], BF16, stack=st) for i in range(2)]
                R_csb = [Res("csb") for _ in range(2)]
                ssi = 0
                steps = [(e, blk) for e in range(NE) for blk in range(NB)]
                pend = None
                cnt = {"u": 0, "w": 0, "c": 0}

                def emit_w2(e, blk, ab):
                    eb = e % 2
                    for m in range(KC):
                        pt, pr = ps_fixed(4 + cnt["w"] % 3)
                        cnt["w"] += 1
                        S.pe_group([lambda pe, jd=jd, pt=pt, m=m: pe.matmul(pt[:], lhsT=wexp[eb][:, 8 + jd, m * 128:(m + 1) * 128], rhs=actb[ab][:, jd, :],
                                                                             start=(jd == 0), stop=(jd == 3)) for jd in range(4)],
                                   reads=[R_wexp[eb], R_actb[ab]], writes=[pr])
                        resid_add(pt, pr, 5, l, b, m, blk)

                nxt = wpieces(1) if NE > 1 else []
                for si_, (e, blk) in enumerate(steps):
                    eb = e % 2
                    ab = si_ % 2
                    bs = slice(blk * 512, (blk + 1) * 512)
                    pc, pcr = ps_fixed(7)
                    c2 = cnt["c"] % 2
                    cnt["c"] += 1
                    S.pe_group([lambda pe, pc=pc, e=e, bs=bs: pe.matmul(pc[:], lhsT=selm[:, e, :], rhs=combT[:, bs], start=True, stop=True)],
                               reads=[R_ct, R_const], writes=[pcr])
                    S.op("act", lambda a, pc=pc, c2=c2: a.copy(out=csb[c2][:], in_=pc[:]), reads=[pcr], writes=[R_csb[c2]])
                    for jd in range(4):
                        u2 = cnt["u"] % 2
                        cnt["u"] += 1
                        pa, par = ps_fixed(2 * u2)
                        pb, pbr = ps_fixed(2 * u2 + 1)
                        S.pe_group([lambda pe, k=k, pa=pa, jd=jd, bs=bs: pe.matmul(pa[:], lhsT=wexp[eb][:, k, jd * 128:(jd + 1) * 128], rhs=hT[:, k, bs],
                                                                                    start=(k == 0), stop=(k == KC - 1)) for k in range(KC)],
                                   reads=[R_wexp[eb], R_h[blk]], writes=[par])
                        S.pe_group([lambda pe, k=k, pb=pb, jd=jd, bs=bs: pe.matmul(pb[:], lhsT=wexp[eb][:, k, DE + jd * 128:DE + (jd + 1) * 128], rhs=hT[:, k, bs],
                                                                                    start=(k == 0), stop=(k == KC - 1)) for k in range(KC)],
                                   reads=[R_wexp[eb], R_h[blk]], writes=[pbr])
                        s3 = ssi % NSS
                        ssi += 1
                        S.op("act", lambda a, pa=pa, s3=s3: a.activation(out=sbuf_s[s3][:], in_=pa[:], func=AF.Silu), reads=[par], writes=[R_ss[s3]])
                        S.op("dve", lambda v, pb=pb, s3=s3: v.tensor_tensor(out=sbuf_s[s3][:], in0=pb[:], in1=sbuf_s[s3][:], op=ALU.mult),
                             reads=[pbr, R_ss[s3]], writes=[R_ss[s3]])
                        S.op("pool", lambda g, c2=c2, s3=s3, jd=jd, ab=ab: g.tensor_tensor(out=actb[ab][:, jd, :], in0=sbuf_s[s3][:], in1=csb[c2][:], op=ALU.mult),
                             reads=[R_csb[c2], R_ss[s3]], writes=[R_actb[ab]])
                        if jd == 0:
                            if pend is not None:
                                emit_w2(*pend)
                                pend = None
                            if blk == 0 and e > 0 and e + 1 < NE:
                                nxt = wpieces(e + 1)
                            for _ in range((6 + NB - 1) // NB):
                                if nxt:
                                    nxt.pop(0)()
                    pend = (e, blk, ab)
                while nxt:
                    nxt.pop(0)()
                emit_w2(*pend)
                cast_mode[0] = "rr"
                S.barrier()

        def final_phase(b):
            with ExitStack() as st:
                sq = [sb("fsq%d" % i, [128, KC, 512], BF16, stack=st) for i in range(2)]
                R_sq = [Res("fsq") for _ in range(2)]
                rstd = [sb("frstd%d" % i, [128, 512], stack=st) for i in range(2)]
                R_rstd = [Res("frstd") for _ in range(2)]
                yn = [sb("yn%d" % i, [128, KC, 512], stack=st) for i in range(2)]
                R_yn = [Res("yn") for _ in range(2)]
                ob = [sb("ob%d" % i, [128, D], stack=st) for i in range(2)]
                R_ob = [Res("ob") for _ in range(2)]
                oi = 0
                for blk in range(NB):
                    bs = slice(blk * 512, (blk + 1) * 512)
                    i2 = blk % 2
                    S.op("act", lambda a, bs=bs, i2=i2: a.activation(out=sq[i2][:], in_=xT[:, :, bs], func=AF.Square),
                         reads=[R_x[blk]], writes=[R_sq[i2]])
                    pt, pr = ps_next()
                    S.pe_group([lambda pe, k=k, pt=pt, i2=i2: pe.matmul(pt[:], lhsT=onesD[:], rhs=sq[i2][:, k, :],
                                                                         start=(k == 0), stop=(k == KC - 1)) for k in range(KC)],
                               reads=[R_sq[i2], R_const], writes=[pr])
                    S.op("act", lambda a, pt=pt, i2=i2: a.activation(out=rstd[i2][:], in_=pt[:], func=AF.Ln, bias=epsc[:, 0:1], scale=1.0),
                         reads=[pr, R_const], writes=[R_rstd[i2]])
                    S.op("act", lambda a, i2=i2: a.activation(out=rstd[i2][:], in_=rstd[i2][:], func=AF.Exp, scale=-0.5), reads=[R_rstd[i2]], writes=[R_rstd[i2]])
                    for k in range(KC):
                        S.op("dve", lambda v, k=k, bs=bs, i2=i2: v.scalar_tensor_tensor(out=yn[i2][:, k, :], in0=xT[:, k, bs], scalar=prm(2, G2["fin"] + k),
                                                                                        in1=rstd[i2][:], op0=ALU.mult, op1=ALU.mult),
                             reads=[R_x[blk], R_rstd[i2], R_prm], writes=[R_yn[i2]])
                    stop_at(7)
                    for tt in range(4):
                        t = blk * 4 + tt
                        o2 = oi % 2
                        oi += 1
                        for half in range(2):
                            pt, pr = ps_next()
                            S.pe_group([lambda pe, kq=kq, half=half, pt=pt, tt=tt, i2=i2: pe.transpose(
                                out=pt[:, kq * 128:(kq + 1) * 128], in_=yn[i2][:, half * 4 + kq, tt * 128:(tt + 1) * 128], identity=ident[:])
                                for kq in range(4)], reads=[R_yn[i2], R_const], writes=[pr])
                            if half == 0:
                                S.op("act", lambda a, pt=pt, o2=o2: a.copy(out=ob[o2][:, 0:512], in_=pt[:]), reads=[pr], writes=[R_ob[o2]])
                            else:
                                S.op("dve", lambda v, pt=pt, o2=o2: v.tensor_copy(out=ob[o2][:, 512:1024], in_=pt[:]), reads=[pr], writes=[R_ob[o2]])
                        stop_at(8)
                        S.dma("sp", ob_ds[o2], out[b, t * 128:(t + 1) * 128, :], ob[o2][:], reads=[R_ob[o2]])
                        stop_at(9)
                S.barrier()

        def load_phase(b):
            with ExitStack() as st:
                xin = [sb("xin%d" % i, [128, D], stack=st) for i in range(2)]
                R_xin = [Res("xin") for _ in range(2)]
                xds = xin_ds
                for t in range(NT):
                    x2 = t % 2
                    S.dma("sp", xds[x2], xin[x2][:], x[b, t * 128:(t + 1) * 128, :], writes=[R_xin[x2]])
                    for half in range(2):
                        pt, pr = ps_next()
                        S.pe_group([lambda pe, kq=kq, half=half, pt=pt, x2=x2: pe.transpose(
                            out=pt[:, kq * 128:(kq + 1) * 128], in_=xin[x2][:, (half * 4 + kq) * 128:(half * 4 + kq + 1) * 128], identity=ident[:])
                            for kq in range(4)], reads=[R_xin[x2], R_const], writes=[pr])
                        dst = xT[:, half * 4:(half + 1) * 4, t * 128:(t + 1) * 128]
                        if half == 0:
                            S.op("act", lambda a, pt=pt, dst=dst: a.copy(out=dst, in_=pt[:].rearrange("p (k t) -> p k t", k=4)),
                                 reads=[pr], writes=[R_x[t // 4]])
                        else:
                            S.op("dve", lambda v, pt=pt, dst=dst: v.tensor_copy(out=dst, in_=pt[:].rearrange("p (k t) -> p k t", k=4)),
                                 reads=[pr], writes=[R_x[t // 4]])
                S.barrier()

        phases = cfg.get("phases", "amr")
        stop_at(5)
        for b in range(NSEQ):
            with nc.named_scope("load"):
                load_phase(b)
            stop_at(6)
            for l in LAYERS:
                if l % 2 == 0:
                    if "a" in phases:
                        with nc.named_scope("attn"):
                            attn_phase(l, b)
                else:
                    if "r" in phases:
                        with nc.named_scope("rec"):
                            rec_phase(l, b)
                if "m" in phases:
                    with nc.named_scope("moe"):
                        moe_phase(l, b)
            if b == 0:
                dbg("xT", xT[:], R_x)
            with nc.named_scope("final"):
                final_phase(b)
        S.barrier()
    except StopBuild:
        pass
    return nc


_CACHE = {}

WNAMES = ["norm_mix", "norm_ffn", "final_norm", "ada_w", "ada_b", "attn_w_qkv", "attn_lambda", "attn_subln", "attn_w_o",
          "rec_w_in", "rec_conv_w", "rec_conv_b", "rec_gate_w", "rec_gate_b", "rec_a_param", "rec_w_out",
          "moe_w_group", "moe_b_group", "moe_w_expert", "moe_b_expert", "moe_w13", "moe_w2"]


def kernel(**inputs):
    x = np.ascontiguousarray(inputs["x"], dtype=np.float32)
    c = np.ascontiguousarray(inputs["c"], dtype=np.float32)
    B, S_, _ = x.shape
    ncores = 8
    nseq = B // ncores
    cfg = {"S": S_, "nseq": nseq, "layers": [0, 1, 2, 3]}
    nc = build(cfg)
    w = {k: np.ascontiguousarray(inputs[k], dtype=np.float32) for k in WNAMES}
    in_maps = []
    for i in range(ncores):
        m = {"x": x[i * nseq:(i + 1) * nseq], "c": c[i * nseq:(i + 1) * nseq]}
        m.update(w)
        in_maps.append(m)
    res = run_bass_kernel_spmd(nc, in_maps, core_ids=list(range(ncores)))
    return np.concatenate([r["out"] for r in res.results], axis=0).astype(np.float32)
```
